# Optimizing a Trainium2 kernel written in Bass

```python
import jax, jax.numpy as jnp
from jax import lax
import numpy as np

D_MODEL = 2048
BATCH = 4
SEQ = 2048
DEPTH = 2

N_B = DEPTH // 2
N_A = DEPTH - N_B
N_DENSE = (DEPTH + 1) // 2
N_MOE = DEPTH // 2
HEAD_DIM = 128
N_HEADS = D_MODEL // HEAD_DIM
CONV_WIDTH = 3
D_FF = 5632
N_EXPERTS = 8
TOP_K = 2
D_FF_EXPERT = 7168
PLE_DIM = 256
Q_BLOCK = 128
RMS_EPS = 1e-6
NEG_BIG = -1e30

kernel_name = "yoco_shortconv_fox_moe_ple"


def rms_norm(x, g):
    xf = x.astype(jnp.float32)
    y = xf * lax.rsqrt(jnp.mean(xf * xf, axis=-1, keepdims=True) + RMS_EPS) * g.astype(jnp.float32)
    return y.astype(x.dtype)


def short_conv_mixer(xn, w_in, w_dw, w_out):
    proj = xn @ w_in
    b_gate, c_gate, x_in = jnp.split(proj, 3, axis=-1)
    u = c_gate * x_in
    kern = w_dw[:, None, :].astype(u.dtype)
    conv = lax.conv_general_dilated(
        u, kern, window_strides=(1,), padding=[(CONV_WIDTH - 1, 0)],
        dimension_numbers=("NWC", "WIO", "NWC"), feature_group_count=D_MODEL)
    return (b_gate * conv) @ w_out


def shared_kv_side(h, kv_norm, w_kvf, b_f, k_norm):
    bsz, seq, _ = h.shape
    xs = rms_norm(h, kv_norm)
    kvf = xs @ w_kvf
    k = kvf[..., :D_MODEL].reshape(bsz, seq, N_HEADS, HEAD_DIM)
    v = kvf[..., D_MODEL:2 * D_MODEL].reshape(bsz, seq, N_HEADS, HEAD_DIM)
    f_logit = (kvf[..., 2 * D_MODEL:] + b_f).astype(jnp.float32)
    k = rms_norm(k, k_norm)
    c = jnp.cumsum(jax.nn.log_sigmoid(f_logit), axis=1)
    return (jnp.transpose(k, (0, 2, 1, 3)), jnp.transpose(v, (0, 2, 1, 3)),
            jnp.transpose(c, (0, 2, 1)))


def forgetting_attention(xn, k, v, c, w_q, q_norm, w_o):
    bsz, seq, _ = xn.shape
    n_blk = seq // Q_BLOCK
    q = rms_norm((xn @ w_q).reshape(bsz, seq, N_HEADS, HEAD_DIM), q_norm)
    q = jnp.transpose(q, (0, 2, 1, 3))
    q_blocks = jnp.moveaxis(q.reshape(bsz, N_HEADS, n_blk, Q_BLOCK, HEAD_DIM), 2, 0)
    c_blocks = jnp.moveaxis(c.reshape(bsz, N_HEADS, n_blk, Q_BLOCK), 2, 0)
    scale = HEAD_DIM ** -0.5
    k_pos = jnp.arange(seq)

    def one_block(args):
        qb, cqb, blk = args
        s = jnp.einsum("bhqd,bhkd->bhqk", qb, k).astype(jnp.float32) * scale
        s = s + cqb[..., :, None] - c[..., None, :]
        q_pos = blk * Q_BLOCK + jnp.arange(Q_BLOCK)
        s = jnp.where(k_pos[None, :] <= q_pos[:, None], s, NEG_BIG)
        a = jax.nn.softmax(s, axis=-1).astype(v.dtype)
        return jnp.einsum("bhqk,bhkd->bhqd", a, v)

    out = lax.map(one_block, (q_blocks, c_blocks, jnp.arange(n_blk)))
    out = jnp.moveaxis(out, 0, 2).reshape(bsz, N_HEADS, seq, HEAD_DIM)
    out = jnp.transpose(out, (0, 2, 1, 3)).reshape(bsz, seq, D_MODEL)
    return out @ w_o


def swiglu(xn, w_gu, w_down):
    g, u = jnp.split(xn @ w_gu, 2, axis=-1)
    return (jax.nn.silu(g) * u) @ w_down


def moe_swiglu(xn, w_router, w_gu, w_down):
    bsz, seq, d = xn.shape
    xf = xn.reshape(bsz * seq, d)
    logits = (xf @ w_router).astype(jnp.float32)
    top_v, top_i = lax.top_k(logits, TOP_K)
    top_w = jax.nn.softmax(top_v, axis=-1)
    gates = jnp.sum(jax.nn.one_hot(top_i, N_EXPERTS, dtype=jnp.float32) * top_w[..., None], axis=1)
    gates = gates.astype(xn.dtype)
    y = jnp.zeros_like(xf)
    for e in range(N_EXPERTS):
        y = y + gates[:, e:e + 1] * swiglu(xf, w_gu[e], w_down[e])
    return y.reshape(bsz, seq, d)


def setup_inputs(seed: int = 0) -> dict:
    key = jax.random.key(seed)
    ks = jax.random.split(key, 24)
    f32 = jnp.float32

    def nrm(k, shape, fan_in):
        return jax.random.normal(k, shape, f32) * (fan_in ** -0.5)

    def gain(k, shape):
        return 1.0 + 0.05 * jax.random.normal(k, shape, f32)

    D = D_MODEL
    return {
        "x": jax.random.normal(ks[0], (BATCH, SEQ, D), f32),
        "p": jax.random.normal(ks[1], (DEPTH, BATCH, SEQ, PLE_DIM), f32),
        "norm_mix": gain(ks[2], (DEPTH, D)),
        "norm_ffn": gain(ks[3], (DEPTH, D)),
        "norm_ple": gain(ks[4], (DEPTH, D)),
        "conv_in_w": nrm(ks[5], (N_A, D, 3 * D), D),
        "conv_dw": nrm(ks[6], (N_A, CONV_WIDTH, D), CONV_WIDTH),
        "conv_out_w": nrm(ks[7], (N_A, D, D), D),
        "kv_norm": gain(ks[8], (D,)),
        "w_kvf": nrm(ks[9], (D, 2 * D + N_HEADS), D),
        "b_f": 3.0 + 0.1 * jax.random.normal(ks[10], (N_HEADS,), f32),
        "k_norm": gain(ks[11], (HEAD_DIM,)),
        "attn_q_w": nrm(ks[12], (N_B, D, D), D),
        "q_norm": gain(ks[13], (N_B, HEAD_DIM)),
        "attn_out_w": nrm(ks[14], (N_B, D, D), D),
        "ffn_gu": nrm(ks[15], (N_DENSE, D, 2 * D_FF), D),
        "ffn_down": nrm(ks[16], (N_DENSE, D_FF, D), D_FF),
        "router_w": nrm(ks[17], (N_MOE, D, N_EXPERTS), D),
        "moe_gu": nrm(ks[18], (N_MOE, N_EXPERTS, D, 2 * D_FF_EXPERT), D),
        "moe_down": nrm(ks[19], (N_MOE, N_EXPERTS, D_FF_EXPERT, D), D_FF_EXPERT),
        "ple_up": nrm(ks[20], (DEPTH, PLE_DIM, D), PLE_DIM),
        "ple_gate": nrm(ks[21], (DEPTH, D, D), D),
    }


def reference(x, p, norm_mix, norm_ffn, norm_ple, conv_in_w, conv_dw, conv_out_w,
              kv_norm, w_kvf, b_f, k_norm, attn_q_w, q_norm, attn_out_w,
              ffn_gu, ffn_down, router_w, moe_gu, moe_down, ple_up, ple_gate):
    h = x
    shared = None
    for i in range(DEPTH):
        xn = rms_norm(h, norm_mix[i])
        if i < N_A:
            h = h + short_conv_mixer(xn, conv_in_w[i], conv_dw[i], conv_out_w[i])
        else:
            if i == N_A:
                shared = shared_kv_side(h, kv_norm, w_kvf, b_f, k_norm)
            j = i - N_A
            k_s, v_s, c_s = shared
            h = h + forgetting_attention(xn, k_s, v_s, c_s, attn_q_w[j], q_norm[j], attn_out_w[j])
        xn = rms_norm(h, norm_ffn[i])
        if i % 2 == 0:
            h = h + swiglu(xn, ffn_gu[i // 2], ffn_down[i // 2])
        else:
            h = h + moe_swiglu(xn, router_w[i // 2], moe_gu[i // 2], moe_down[i // 2])
        gate = jax.nn.sigmoid((rms_norm(h, norm_ple[i]) @ ple_gate[i]).astype(jnp.float32))
        h = h + gate.astype(h.dtype) * (p[i] @ ple_up[i])
    return h
```

```python
import numpy as np
from contextlib import ExitStack
import concourse.bass as bass
import concourse.mybir as mybir
from concourse.bass_utils import run_bass_kernel_spmd

F32 = mybir.dt.float32
BF16 = mybir.dt.bfloat16
ALU = mybir.AluOpType
AF = mybir.ActivationFunctionType
AX = mybir.AxisListType

D = 2048
KC = 16
TN = 1024
NH = 16
DFF = 5632
FQ = 11
NE = 8
DFE = 7168
EQ = 14
NS = 5
NTMP = 5
EPS = 1e-6
SCALE = 128 ** -0.5
NVEC = 179
V_MIX0, V_FFN0, V_PLE0, V_KV, V_MIX1, V_FFN1, V_PLE1, V_CW, V_KN, V_QN, V_VIS, V_BF = 0, 16, 32, 48, 64, 80, 96, 112, 160, 161, 162, 163


class Buf:
    __slots__ = ("name", "w", "rs")

    def __init__(self, name):
        self.name = name
        self.w = None
        self.rs = []


class Trk:
    def __init__(self, nc, stack):
        self.nc = nc
        self.stack = stack
        self.engs = {"pe": nc.tensor, "act": nc.scalar, "dve": nc.vector, "pool": nc.gpsimd, "sp": nc.sync}
        self.sem = {}
        self.cnt = {}
        self.owner = {}
        self.seen = {k: {} for k in self.engs}
        self.nsem = 0
        for k in self.engs:
            self._new_sem(k)
        self.dsem = {}

    def _new_sem(self, k):
        s = self.stack.enter_context(self.nc.semaphore(f"s_{k}_{self.nsem}"))
        self.nsem += 1
        self.sem[k] = s
        self.cnt[k] = 0
        self.owner[id(s)] = k

    def roll(self):
        for k in ("pe", "act", "dve"):
            self._new_sem(k)

    def _waits(self, e, reads, writes):
        need = {}

        def add(st):
            if st is None:
                return
            s, v = st
            if e == "pe" and self.owner.get(id(s)) == "pe":
                return
            key = id(s)
            if key not in need or need[key][1] < v:
                need[key] = (s, v)
        for b in reads:
            add(b.w)
        for b in writes:
            add(b.w)
            for r in b.rs:
                add(r)
        eng = self.engs[e]
        seen = self.seen[e]
        for key, (s, v) in need.items():
            if seen.get(key, 0) < v:
                eng.wait_ge(s, v)
                seen[key] = v

    @staticmethod
    def _stamp(st, reads, writes):
        for b in reads:
            b.rs.append(st)
            if len(b.rs) > 64:
                last = {}
                for s, v in b.rs:
                    if id(s) not in last or last[id(s)][1] < v:
                        last[id(s)] = (s, v)
                b.rs = list(last.values())
        for b in writes:
            b.w = st
            b.rs = []

    def op(self, e, fn, reads=(), writes=()):
        self._waits(e, reads, writes)
        ins = fn(self.engs[e])
        self.cnt[e] += 1
        ins.then_inc(self.sem[e], 1)
        self._stamp((self.sem[e], self.cnt[e]), reads, writes)

    def dma(self, e, key, out, in_, reads=(), writes=()):
        if key not in self.dsem:
            s = self.stack.enter_context(self.nc.semaphore(f"d_{key}"))
            self.dsem[key] = [s, 0]
        s, c = self.dsem[key]
        eng = self.engs[e]
        seen = self.seen[e]
        if c > 0 and seen.get(id(s), 0) < c:
            eng.wait_ge(s, c)
            seen[id(s)] = c
        self._waits(e, reads, writes)
        eng.dma_start(out=out, in_=in_).then_inc(s, 16)
        self.dsem[key][1] = c + 16
        self._stamp((s, c + 16), reads, writes)

    def finish(self, e, bufs):
        self._waits(e, bufs, ())


def build_nc(n_experts=NE, dbg=False):
    nc = bass.Bass("TRN2", target_bir_lowering=False)

    def din(name, shape, dt=F32):
        return nc.dram_tensor(name, list(shape), dt, kind="ExternalInput").ap()

    xT = din("xT", [128, KC, 2 * TN])
    pT0 = din("pT0", [128, 2, 2 * TN])
    pT1 = din("pT1", [128, 2, TN])
    vecs = din("vecs", [128, NVEC])
    consts = din("consts", [128, 3, 128])
    wr = din("wr", [128, KC, NE])
    w_cin = din("w_cin", [48, 128, KC, 128])
    w_cout = din("w_cout", [16, 128, KC, 128])
    w_gu = din("w_gu", [88, 128, KC, 128])
    w_dn = din("w_dn", [4, 16, 128, FQ, 128])
    w_pg = din("w_pg", [2, 16, 128, KC, 128])
    w_pu = din("w_pu", [2, 16, 128, 2, 128])
    w_k = din("w_k", [16, 128, KC, 128])
    w_v = din("w_v", [16, 128, KC, 128])
    w_f = din("w_f", [128, KC, 16])
    w_q = din("w_q", [16, 128, KC, 128])
    w_o = din("w_o", [16, 128, KC, 128])
    if n_experts > 0:
        w_mgu = din("w_mgu", [NE, 112, 128, KC, 128])
        w_mdn = din("w_mdn", [NE, 4, 16, 128, EQ, 128])
    out = nc.dram_tensor("out", [128, KC, TN], F32, kind="ExternalOutput").ap()
    ktd = nc.dram_tensor("ktd", [NH, 128, 2 * TN], BF16, kind="Internal").ap()
    vd = nc.dram_tensor("vd", [NH, 128, 16, 128], BF16, kind="Internal").ap()
    if dbg:
        dbg_o = nc.dram_tensor("dbg", [4, 128, KC, TN], F32, kind="ExternalOutput").ap()

    with ExitStack() as st:
        def sb(name, shape, dt):
            return st.enter_context(nc.sbuf_tensor(name, list(shape), dt))

        H = sb("H", [128, KC, TN], F32)
        XN = sb("XN", [128, KC, TN], BF16)
        BIG = sb("BIG", [128, KC, TN], BF16)
        WS = sb("WS", [128, NS, KC * 128], BF16)
        SCR = sb("SCR", [128, 12288], BF16)
        TMP = sb("TMP", [128, NTMP, TN + 2], F32)
        VEC = sb("VEC", [128, NVEC], F32)
        WR = sb("WR", [128, KC, NE], F32)
        CON = sb("CON", [128, 3, 128], F32)
        CMD = sb("CMD", [128, 128], F32)
        CMH = sb("CMH", [128, 128], F32)
        ONESF = sb("ONESF", [128, 128], F32)
        ONESB = sb("ONESB", [128, 128], BF16)
        TRIB = sb("TRIB", [128, 128], BF16)
        EPSC = sb("EPSC", [128, 1], F32)
        HALO = sb("HALO", [128, KC, 2], F32)
        LS = sb("LS", [128, 16, 16], F32)
        CC = sb("CC", [128, 16, 16], F32)
        BIASQ = sb("BIASQ", [128, 2, 16, 16], F32)
        FL = sb("FL", [128, 8, 16], F32)
        LG = sb("LG", [128, 8, 8], F32)
        LG2 = sb("LG2", [128, 8, 8], F32)
        EQ1 = sb("EQ1", [128, 8, 8], F32)
        EQ2 = sb("EQ2", [128, 8, 8], F32)
        GT = sb("GT", [128, 8, 8], F32)
        MM = sb("MM", [128, 8, 4], F32)
        DG = sb("DG", [128, 2, 128], F32)
        PS = st.enter_context(nc.psum_tensor("PS", [128, 4096], F32))

        IDENT, TRI, SEL = CON[:, 0, :], CON[:, 1, :], CON[:, 2, :]
        tk = Trk(nc, st)

        bH = [Buf(f"H{j}") for j in range(KC)]
        bXN = [Buf(f"XN{j}") for j in range(KC)]
        bBIG = [Buf(f"BIG{j}") for j in range(KC)]
        bWS = [Buf(f"WS{s}") for s in range(NS)]
        bT = [Buf(f"T{i}") for i in range(NTMP)]
        bP = [Buf(f"P{i}") for i in range(8)]
        bVEC, bWR, bCON, bCST, bHALO, bLS, bCC, bBQ, bFL = (Buf(n) for n in ("VEC", "WR", "CON", "CST", "HALO", "LS", "CC", "BQ", "FL"))
        bSCR = [Buf(f"SCR{i}") for i in range(12)]
        bKD, bVD, bOUT, bDBG, bRT, bDG = Buf("KD"), Buf("VD"), Buf("OUT"), Buf("DBG"), Buf("RT"), [Buf("DG0"), Buf("DG1")]

        st_ = {"wi": 0, "pi": 0, "bi": 0, "ti": 0}

        def wload(src, kc, width=128):
            s = st_["wi"] % NS
            st_["wi"] += 1
            dst = WS[:, s, 0:kc * width].rearrange("p (k m) -> p k m", k=kc)
            tk.dma("pool", f"ws{s}", dst, src, writes=[bWS[s]])
            return s

        def wsv(s, k, width=128):
            return WS[:, s, k * width:(k + 1) * width]

        def pair(n=3):
            p = st_["pi"] % n
            st_["pi"] += 1
            return p

        def bank(n=6):
            b = st_["bi"] % n
            st_["bi"] += 1
            return b

        def tmp():
            t = st_["ti"] % NTMP
            st_["ti"] += 1
            return t

        def PP(p):
            return PS[:, p * 1024:(p + 1) * 1024]

        def PB(b, lo=0, hi=512):
            return PS[:, b * 512 + lo:b * 512 + hi]

        def bPP(p):
            return [bP[2 * p], bP[2 * p + 1]]

        def T(t, lo=0, hi=TN):
            return TMP[:, t, lo:hi]

        def vcol(c):
            return VEC[:, c:c + 1]

        def fm_group(p, s, kc, rhs, reads):
            def f(e):
                ins = None
                for half in range(2):
                    for k in range(kc):
                        ins = e.matmul(PB(2 * p + half), wsv(s, k), rhs(k, half), start=(k == 0), stop=(k == kc - 1))
                return ins
            tk.op("pe", f, reads=[bWS[s]] + list(reads), writes=bPP(p))

        def xn_rhs(k, half):
            return XN[:, k, half * 512:(half + 1) * 512]

        def big_rhs(k, half):
            return BIG[:, k, half * 512:(half + 1) * 512]

        def bcast_mean(p, src_t, cm):
            def f(e):
                ins = None
                for half in range(2):
                    ins = e.matmul(PB(2 * p + half), cm[:], T(src_t, half * 512, (half + 1) * 512), start=True, stop=True)
                return ins
            tk.op("pe", f, reads=[bT[src_t], bCST], writes=bPP(p))

        def rstd_from(p):
            r = tmp()
            tk.op("act", lambda e: e.activation(out=T(r), in_=PP(p), func=AF.Sqrt, bias=EPSC[:, 0:1], scale=1.0),
                  reads=bPP(p) + [bCST], writes=[bT[r]])
            tk.op("dve", lambda e: e.reciprocal(out=T(r), in_=T(r)), reads=[bT[r]], writes=[bT[r]])
            return r

        def rmsnorm(gc, router=False):
            p = pair()
            for j in range(KC):
                t = tmp()
                tk.op("act", lambda e: e.activation(out=T(t), in_=H[:, j, :], func=AF.Square), reads=[bH[j]], writes=[bT[t]])

                def f(e):
                    ins = None
                    for half in range(2):
                        ins = e.matmul(PB(2 * p + half), CMD[:], T(t, half * 512, (half + 1) * 512), start=(j == 0), stop=(j == KC - 1))
                    return ins
                tk.op("pe", f, reads=[bT[t], bCST], writes=bPP(p))
            r = rstd_from(p)
            if not router:
                for j in range(KC):
                    tk.op("dve", lambda e: e.scalar_tensor_tensor(out=XN[:, j, :], in0=H[:, j, :], scalar=vcol(gc + j), in1=T(r),
                                                                   op0=ALU.mult, op1=ALU.mult),
                          reads=[bH[j], bVEC, bT[r]], writes=[bXN[j]])
                return None
            lb = 7
            for j in range(KC):
                t = tmp()
                if t == r:
                    t = tmp()
                tk.op("dve", lambda e: e.scalar_tensor_tensor(out=T(t), in0=H[:, j, :], scalar=vcol(gc + j), in1=T(r),
                                                               op0=ALU.mult, op1=ALU.mult),
                      reads=[bH[j], bVEC, bT[r]], writes=[bT[t]])
                tk.op("act", lambda e: e.activation(out=XN[:, j, :], in_=T(t), func=AF.Copy), reads=[bT[t]], writes=[bXN[j]])

                def f(e):
                    ins = None
                    for tb in range(8):
                        ins = e.matmul(PB(lb, tb * 8, tb * 8 + 8), T(t, tb * 128, (tb + 1) * 128), WR[:, j, :],
                                       start=(j == 0 and tb == 0), stop=(j == KC - 1), skip_group_check=True)
                    return ins
                tk.op("pe", f, reads=[bT[t], bWR], writes=[bP[lb]])
            return lb

        def add_into_H(j, p):
            tk.op("dve", lambda e: e.tensor_tensor(out=H[:, j, :], in0=PP(p), in1=H[:, j, :], op=ALU.add), reads=bPP(p) + [bH[j]], writes=[bH[j]])

        def barrier():
            stamps = [(tk.sem[k], tk.cnt[k]) for k in ("pe", "act", "dve") if tk.cnt[k] > 0]
            stamps += [(s, c) for (s, c) in tk.dsem.values() if c > 0]
            fake = Buf("bar")
            for e in ("pe", "act", "dve", "sp", "pool"):
                for stp in stamps:
                    fake.w = stp
                    tk._waits(e, [fake], ())

        tk.dma("sp", "vec", VEC[:], vecs, writes=[bVEC])
        tk.dma("sp", "con", CON[:], consts, writes=[bCON])
        tk.dma("sp", "wr", WR[:], wr, writes=[bWR])
        tk.op("dve", lambda e: e.memset(CMD[:], 1.0 / D), writes=[bCST])
        tk.op("dve", lambda e: e.memset(CMH[:], 1.0 / 128), writes=[bCST])
        tk.op("dve", lambda e: e.memset(ONESF[:], 1.0), writes=[bCST])
        tk.op("dve", lambda e: e.memset(ONESB[:], 1.0), writes=[bCST])
        tk.op("dve", lambda e: e.memset(EPSC[:], EPS), writes=[bCST])
        tk.op("dve", lambda e: e.memset(HALO[:], 0.0), writes=[bHALO])
        tk.op("dve", lambda e: e.tensor_copy(out=TRIB[:], in_=TRI), reads=[bCON], writes=[bCST])

        PT = SCR[:, 0:2048].rearrange("p (a b) -> p a b", a=2)

        def ple(layer, gc):
            rmsnorm(gc)
            for j in range(KC):
                sg = wload(w_pg[layer, j], KC)
                su = wload(w_pu[layer, j], 2)
                pa, pb = pair(), pair()
                fm_group(pa, sg, KC, xn_rhs, bXN)
                fm_group(pb, su, 2, lambda k, half: PT[:, k, half * 512:(half + 1) * 512], [bSCR[0], bSCR[1]])
                a, b = tmp(), tmp()
                tk.op("act", lambda e: e.activation(out=T(a), in_=PP(pa), func=AF.Sigmoid), reads=bPP(pa), writes=[bT[a]])
                tk.op("dve", lambda e: e.tensor_tensor(out=T(b), in0=T(a), in1=PP(pb), op=ALU.mult), reads=[bT[a]] + bPP(pb), writes=[bT[b]])
                tk.op("dve", lambda e: e.tensor_tensor(out=H[:, j, :], in0=T(b), in1=H[:, j, :], op=ALU.add), reads=[bT[b], bH[j]], writes=[bH[j]])

        def layer0_pass(ps_):
            c0 = ps_ * TN
            for j in range(KC):
                tk.dma("sp", f"h{j % 4}", H[:, j, :], xT[:, j, c0:c0 + TN], writes=[bH[j]])
            tk.dma("pool", "pt", PT, pT0[:, :, c0:c0 + TN], writes=[bSCR[0], bSCR[1]])
            rmsnorm(V_MIX0)
            for j in range(KC):
                sC = wload(w_cin[16 + j], KC)
                sX = wload(w_cin[32 + j], KC)
                sB = wload(w_cin[j], KC)
                pC, pX, pBg = pair(), pair(), pair()
                fm_group(pC, sC, KC, xn_rhs, bXN)
                fm_group(pX, sX, KC, xn_rhs, bXN)
                fm_group(pBg, sB, KC, xn_rhs, bXN)
                a, u, acc = tmp(), tmp(), tmp()
                tk.op("act", lambda e: e.activation(out=T(a), in_=PP(pC), func=AF.Copy), reads=bPP(pC), writes=[bT[a]])
                tk.op("dve", lambda e: e.tensor_copy(out=TMP[:, u, 0:2], in_=HALO[:, j, :]), reads=[bHALO], writes=[bT[u]])
                tk.op("dve", lambda e: e.tensor_tensor(out=TMP[:, u, 2:TN + 2], in0=T(a), in1=PP(pX), op=ALU.mult),
                      reads=[bT[a]] + bPP(pX), writes=[bT[u]])
                tk.op("dve", lambda e: e.tensor_copy(out=HALO[:, j, :], in_=TMP[:, u, TN:TN + 2]), reads=[bT[u]], writes=[bHALO])
                tk.op("dve", lambda e: e.tensor_scalar(out=T(acc), in0=TMP[:, u, 0:TN], scalar1=vcol(V_CW + j), scalar2=None, op0=ALU.mult),
                      reads=[bT[u], bVEC], writes=[bT[acc]])
                for i in (1, 2):
                    tk.op("dve", lambda e: e.scalar_tensor_tensor(out=T(acc), in0=TMP[:, u, i:TN + i], scalar=vcol(V_CW + 16 * i + j), in1=T(acc),
                                                                   op0=ALU.mult, op1=ALU.add),
                          reads=[bT[u], bVEC, bT[acc]], writes=[bT[acc]])
                tk.op("dve", lambda e: e.tensor_tensor(out=BIG[:, j, :], in0=T(acc), in1=PP(pBg), op=ALU.mult),
                      reads=[bT[acc]] + bPP(pBg), writes=[bBIG[j]])
            for j in range(KC):
                s = wload(w_cout[j], KC)
                p = pair()
                fm_group(p, s, KC, big_rhs, bBIG)
                add_into_H(j, p)
            rmsnorm(V_FFN0)
            for q in range(4):
                for fi in range(FQ):
                    f = q * FQ + fi
                    sg = wload(w_gu[f], KC)
                    su = wload(w_gu[44 + f], KC)
                    pg, pu = pair(), pair()
                    fm_group(pg, sg, KC, xn_rhs, bXN)
                    fm_group(pu, su, KC, xn_rhs, bXN)
                    a = tmp()
                    tk.op("act", lambda e: e.activation(out=T(a), in_=PP(pg), func=AF.Silu), reads=bPP(pg), writes=[bT[a]])
                    tk.op("dve", lambda e: e.tensor_tensor(out=BIG[:, fi, :], in0=T(a), in1=PP(pu), op=ALU.mult),
                          reads=[bT[a]] + bPP(pu), writes=[bBIG[fi]])
                for j in range(KC):
                    s = wload(w_dn[q, j], FQ)
                    p = pair()
                    fm_group(p, s, FQ, big_rhs, bBIG[:FQ])
                    add_into_H(j, p)
            ple(0, V_PLE0)
            if dbg:
                tk.dma("sp", "dbg", dbg_o[ps_], H[:], reads=bH, writes=[bDBG])
            rmsnorm(V_KV)
            for hd in range(NH):
                s = wload(w_k[hd], KC)
                p = pair()
                fm_group(p, s, KC, xn_rhs, bXN)
                a = tmp()
                tk.op("act", lambda e: e.activation(out=T(a), in_=PP(p), func=AF.Square), reads=bPP(p), writes=[bT[a]])
                p2 = pair()
                bcast_mean(p2, a, CMH)
                r = rstd_from(p2)
                tk.op("dve", lambda e: e.scalar_tensor_tensor(out=BIG[:, hd, :], in0=PP(p), scalar=vcol(V_KN), in1=T(r), op0=ALU.mult, op1=ALU.mult),
                      reads=bPP(p) + [bVEC, bT[r]], writes=[bBIG[hd]])
                tk.dma("sp", f"ko{hd % 4}", ktd[hd, :, c0:c0 + TN], BIG[:, hd, :], reads=[bBIG[hd]], writes=[bKD])
            for j in range(NH):
                s = wload(w_v[j], KC)
                p = pair()

                def f(e):
                    ins = None
                    for tb in range(8):
                        for k in range(KC):
                            ins = e.matmul(PS[:, p * 1024 + tb * 128:p * 1024 + (tb + 1) * 128], XN[:, k, tb * 128:(tb + 1) * 128], wsv(s, k),
                                           start=(k == 0), stop=(k == KC - 1))
                    return ins
                tk.op("pe", f, reads=[bWS[s]] + bXN, writes=bPP(p))
                tk.op("act", lambda e: e.activation(out=BIG[:, j, :], in_=PP(p), func=AF.Copy), reads=bPP(p), writes=[bBIG[j]])
                tk.dma("sp", f"vo{j % 4}", vd[j, :, ps_ * 8:(ps_ + 1) * 8, :], BIG[:, j, :].rearrange("p (a b) -> p a b", a=8),
                       reads=[bBIG[j]], writes=[bVD])
            s = wload(w_f, KC, width=16)
            fb = 6

            def f(e):
                ins = None
                for tb in range(8):
                    for k in range(KC):
                        ins = e.matmul(PB(fb, tb * 16, tb * 16 + 16), XN[:, k, tb * 128:(tb + 1) * 128], wsv(s, k, 16), start=(k == 0), stop=(k == KC - 1))
                return ins
            tk.op("pe", f, reads=[bWS[s]] + bXN, writes=[bP[fb]])
            for tb in range(8):
                tk.op("dve", lambda e: e.tensor_tensor(out=FL[:, tb, :], in0=PB(fb, tb * 16, tb * 16 + 16), in1=VEC[:, V_BF:V_BF + 16], op=ALU.add),
                      reads=[bP[fb], bVEC], writes=[bFL])
            tk.op("act", lambda e: e.activation(out=FL[:], in_=FL[:], func=AF.Sigmoid), reads=[bFL], writes=[bFL])
            tk.op("act", lambda e: e.activation(out=LS[:, ps_ * 8:(ps_ + 1) * 8, :], in_=FL[:], func=AF.Ln), reads=[bFL], writes=[bLS])

        layer0_pass(0)
        tk.roll()
        layer0_pass(1)
        tk.roll()

        cb = 6
        for b in range(16):
            def f(e):
                ins = None
                for b2 in range(b + 1):
                    ins = e.matmul(PB(cb, 0, 16), (ONESF[:] if b2 < b else TRI), LS[:, b2, :], start=(b2 == 0), stop=(b2 == b))
                return ins
            tk.op("pe", f, reads=[bLS, bCST, bCON], writes=[bP[cb]])
            tk.op("act", lambda e: e.activation(out=CC[:, b, :], in_=PB(cb, 0, 16), func=AF.Copy), reads=[bP[cb]], writes=[bCC])
        for qt in range(2):
            lastb = 8 + 4 * qt + 3
            tk.op("pe", lambda e: e.matmul(PB(cb, 0, 16), SEL, CC[:, lastb, :], start=True, stop=True), reads=[bCC, bCON], writes=[bP[cb]])
            for kb in range(16):
                tk.op("dve", lambda e: e.tensor_tensor(out=BIASQ[:, qt, kb, :], in0=PB(cb, 0, 16), in1=CC[:, kb, :], op=ALU.subtract),
                      reads=[bP[cb], bCC], writes=[bBQ])
            tk.op("dve", lambda e: e.tensor_scalar(out=BIASQ[:, qt, 0:8, :], in0=BIASQ[:, qt, 0:8, :], scalar1=vcol(V_VIS), scalar2=None, op0=ALU.add),
                  reads=[bBQ, bVEC], writes=[bBQ])
        rmsnorm(V_MIX1)
        barrier()

        def KTH(i):
            return SCR[:, i * 2048:(i + 1) * 2048]

        def VH(i):
            return SCR[:, 4096 + i * 2048:4096 + (i + 1) * 2048]

        def QTH(i):
            return SCR[:, 8192 + i * 1024:8192 + (i + 1) * 1024]

        def PTL(i):
            return SCR[:, 10240 + i * 512:10240 + (i + 1) * 512]

        bKTH = [[bSCR[0], bSCR[1]], [bSCR[2], bSCR[3]]]
        bVH = [[bSCR[4], bSCR[5]], [bSCR[6], bSCR[7]]]
        bQTH = [[bSCR[8]], [bSCR[9]]]
        bPTL = [Buf(f"PT{i}") for i in range(4)]
        OB, LB = 6, 7
        pti = 0
        for h in range(NH):
            i2 = h % 2
            s = wload(w_q[h], KC)
            p = pair()
            fm_group(p, s, KC, xn_rhs, bXN)
            a = tmp()
            tk.op("act", lambda e: e.activation(out=T(a), in_=PP(p), func=AF.Square), reads=bPP(p), writes=[bT[a]])
            p2 = pair()
            bcast_mean(p2, a, CMH)
            r = rstd_from(p2)
            tk.op("dve", lambda e: e.scalar_tensor_tensor(out=QTH(i2), in0=PP(p), scalar=vcol(V_QN), in1=T(r), op0=ALU.mult, op1=ALU.mult),
                  reads=bPP(p) + [bVEC, bT[r]], writes=bQTH[i2])
            tk.dma("sp", f"kl{i2}", KTH(i2), ktd[h], reads=[bKD], writes=bKTH[i2])
            tk.dma("sp", f"vl{i2}", VH(i2).rearrange("p (a b) -> p a b", a=16), vd[h], reads=[bVD], writes=bVH[i2])
            for qt in range(2):
                nkb = 8 + 4 * qt + 4
                for kb in range(nkb):
                    jd = kb - (8 + 4 * qt)
                    lo = 128 * jd if jd >= 0 else 0
                    sbk = bank()
                    tk.op("pe", lambda e: e.matmul(PB(sbk, lo, 512), KTH(i2)[:, kb * 128:(kb + 1) * 128], QTH(i2)[:, qt * 512 + lo:(qt + 1) * 512],
                                                   start=True, stop=True),
                          reads=bKTH[i2] + bQTH[i2], writes=[bP[sbk]])
                    pt = pti % 4
                    pti += 1
                    tk.op("act", lambda e: e.activation(out=PTL(pt)[:, lo:512], in_=PB(sbk, lo, 512), func=AF.Exp,
                                                        bias=BIASQ[:, qt, kb, h:h + 1], scale=SCALE),
                          reads=[bP[sbk], bBQ], writes=[bPTL[pt]])
                    if jd >= 0:
                        tk.op("dve", lambda e: e.tensor_tensor(out=PTL(pt)[:, lo:lo + 128], in0=PTL(pt)[:, lo:lo + 128], in1=TRIB[:], op=ALU.mult),
                              reads=[bPTL[pt], bCST], writes=[bPTL[pt]])

                    def f(e):
                        e.matmul(PB(OB, lo, 512), VH(i2)[:, kb * 128:(kb + 1) * 128], PTL(pt)[:, lo:512], start=(kb == 0), stop=(kb == nkb - 1))
                        return e.matmul(PB(LB, lo, 512), ONESB[:], PTL(pt)[:, lo:512], start=(kb == 0), stop=(kb == nkb - 1))
                    tk.op("pe", f, reads=bVH[i2] + [bPTL[pt], bCST], writes=[bP[OB], bP[LB]])
                r = tmp()
                tk.op("dve", lambda e: e.reciprocal(out=T(r, 0, 512), in_=PB(LB)), reads=[bP[LB]], writes=[bT[r]])
                tk.op("dve", lambda e: e.tensor_tensor(out=BIG[:, h, qt * 512:(qt + 1) * 512], in0=PB(OB), in1=T(r, 0, 512), op=ALU.mult),
                      reads=[bP[OB], bT[r]], writes=[bBIG[h]])
        for j in range(KC):
            s = wload(w_o[j], KC)
            p = pair()
            fm_group(p, s, KC, big_rhs, bBIG)
            add_into_H(j, p)
        if dbg:
            tk.dma("sp", "dbg", dbg_o[2], H[:], reads=bH, writes=[bDBG])
        tk.roll()
        barrier()

        lb = rmsnorm(V_FFN1, router=True)
        tk.op("act", lambda e: e.activation(out=LG[:], in_=PB(lb, 0, 64).rearrange("p (a b) -> p a b", a=8), func=AF.Copy), reads=[bP[lb]], writes=[bRT])
        for tb in range(8):
            def rt(fn):
                tk.op("dve", fn, reads=[bRT], writes=[bRT])
            rt(lambda e: e.tensor_reduce(out=MM[:, tb, 0:1], in_=LG[:, tb, :], axis=AX.X, op=ALU.max))
            rt(lambda e: e.tensor_scalar(out=EQ1[:, tb, :], in0=LG[:, tb, :], scalar1=MM[:, tb, 0:1], scalar2=None, op0=ALU.is_equal))
            rt(lambda e: e.scalar_tensor_tensor(out=LG2[:, tb, :], in0=EQ1[:, tb, :], scalar=-1e30, in1=LG[:, tb, :], op0=ALU.mult, op1=ALU.add))
            rt(lambda e: e.tensor_reduce(out=MM[:, tb, 1:2], in_=LG2[:, tb, :], axis=AX.X, op=ALU.max))
            rt(lambda e: e.tensor_scalar(out=EQ2[:, tb, :], in0=LG2[:, tb, :], scalar1=MM[:, tb, 1:2], scalar2=None, op0=ALU.is_equal))
            rt(lambda e: e.tensor_tensor(out=MM[:, tb, 2:3], in0=MM[:, tb, 1:2], in1=MM[:, tb, 0:1], op=ALU.subtract))
        tk.op("act", lambda e: e.activation(out=MM[:, :, 2:3], in_=MM[:, :, 2:3], func=AF.Sigmoid), reads=[bRT], writes=[bRT])
        tk.op("dve", lambda e: e.tensor_scalar(out=MM[:, :, 3:4], in0=MM[:, :, 2:3], scalar1=-1.0, scalar2=1.0, op0=ALU.mult, op1=ALU.add),
              reads=[bRT], writes=[bRT])
        for tb in range(8):
            tk.op("dve", lambda e: e.tensor_scalar(out=GT[:, tb, :], in0=EQ1[:, tb, :], scalar1=MM[:, tb, 3:4], scalar2=None, op0=ALU.mult),
                  reads=[bRT], writes=[bRT])
            tk.op("dve", lambda e: e.scalar_tensor_tensor(out=GT[:, tb, :], in0=EQ2[:, tb, :], scalar=MM[:, tb, 2:3], in1=GT[:, tb, :],
                                                           op0=ALU.mult, op1=ALU.add),
                  reads=[bRT], writes=[bRT])

        def GATE(e_):
            return SCR[:, e_ * 1024:(e_ + 1) * 1024]

        di = 0
        for e_ in range(n_experts):
            p = pair()
            for tb in range(8):
                d_ = di % 2
                di += 1
                tk.op("dve", lambda e: e.tensor_scalar(out=DG[:, d_, :], in0=IDENT, scalar1=GT[:, tb, e_:e_ + 1], scalar2=None, op0=ALU.mult),
                      reads=[bRT, bCON], writes=[bDG[d_]])
                tk.op("pe", lambda e: e.matmul(PS[:, p * 1024 + tb * 128:p * 1024 + (tb + 1) * 128], ONESF[:], DG[:, d_, :], start=True, stop=True),
                      reads=[bDG[d_], bCST], writes=bPP(p))
            tk.op("act", lambda e: e.activation(out=GATE(e_), in_=PP(p), func=AF.Copy), reads=bPP(p), writes=[bSCR[e_]])
        for e_ in range(n_experts):
            for q in range(4):
                for fi in range(EQ):
                    f_ = q * EQ + fi
                    sg = wload(w_mgu[e_, f_], KC)
                    su = wload(w_mgu[e_, 56 + f_], KC)
                    pg, pu = pair(), pair()
                    fm_group(pg, sg, KC, xn_rhs, bXN)
                    fm_group(pu, su, KC, xn_rhs, bXN)
                    a, b = tmp(), tmp()
                    tk.op("act", lambda e: e.activation(out=T(a), in_=PP(pg), func=AF.Silu), reads=bPP(pg), writes=[bT[a]])
                    tk.op("dve", lambda e: e.tensor_tensor(out=T(b), in0=T(a), in1=PP(pu), op=ALU.mult), reads=[bT[a]] + bPP(pu), writes=[bT[b]])
                    tk.op("dve", lambda e: e.tensor_tensor(out=BIG[:, fi, :], in0=T(b), in1=GATE(e_), op=ALU.mult),
                          reads=[bT[b], bSCR[e_]], writes=[bBIG[fi]])
                for j in range(KC):
                    s = wload(w_mdn[e_, q, j], EQ)
                    p = pair()
                    fm_group(p, s, EQ, big_rhs, bBIG[:EQ])
                    add_into_H(j, p)
            tk.roll()
        if dbg:
            tk.dma("sp", "dbg", dbg_o[3], H[:], reads=bH, writes=[bDBG])
        barrier()

        tk.dma("pool", "pt", PT, pT1, writes=[bSCR[0], bSCR[1]])
        ple(1, V_PLE1)
        for j in range(KC):
            tk.dma("sp", f"h{j % 4}", out[:, j, :], H[:, j, :], reads=[bH[j]], writes=[bOUT])
        tk.finish("sp", [bOUT] + ([bDBG] if dbg else []))
    return nc


def _wtile(W):
    K, N = W.shape
    return np.ascontiguousarray(W.reshape(K // 128, 128, N // 128, 128).transpose(2, 1, 0, 3))


def _fm(a):
    t, f = a.shape
    return np.ascontiguousarray(a.T.reshape(f // 128, 128, t).transpose(1, 0, 2))


def _cols(v):
    return np.asarray(v, np.float32).reshape(-1, 128).T


def prep_shared(inp):
    f32 = np.float32
    g = {k: np.asarray(v, f32) for k, v in inp.items()}
    sh = {}
    sh["w_cin"] = _wtile(g["conv_in_w"][0])
    sh["w_cout"] = _wtile(g["conv_out_w"][0])
    sh["w_gu"] = _wtile(g["ffn_gu"][0])
    dn = g["ffn_down"][0]
    sh["w_dn"] = np.stack([_wtile(dn[q * FQ * 128:(q + 1) * FQ * 128]) for q in range(4)])
    sh["w_pg"] = np.stack([_wtile(g["ple_gate"][i]) for i in range(2)])
    sh["w_pu"] = np.stack([_wtile(g["ple_up"][i]) for i in range(2)])
    wkvf = g["w_kvf"]
    sh["w_k"] = _wtile(wkvf[:, :D])
    sh["w_v"] = _wtile(wkvf[:, D:2 * D])
    sh["w_f"] = np.ascontiguousarray(wkvf[:, 2 * D:].reshape(KC, 128, 16).transpose(1, 0, 2))
    sh["w_q"] = _wtile(g["attn_q_w"][0])
    sh["w_o"] = _wtile(g["attn_out_w"][0])
    sh["w_mgu"] = np.stack([_wtile(g["moe_gu"][0, e]) for e in range(NE)])
    md = g["moe_down"][0]
    sh["w_mdn"] = np.stack([np.stack([_wtile(md[e, q * EQ * 128:(q + 1) * EQ * 128]) for q in range(4)]) for e in range(NE)])
    sh["wr"] = np.ascontiguousarray(g["router_w"][0].reshape(KC, 128, NE).transpose(1, 0, 2))
    ident = np.eye(128, dtype=f32)
    tri = np.triu(np.ones((128, 128), f32))
    sel = np.zeros((128, 128), f32)
    sel[127, :] = 1.0
    sh["consts"] = np.ascontiguousarray(np.stack([ident, tri, sel], axis=1))
    vec = np.zeros((128, NVEC), f32)
    vec[:, V_MIX0:V_MIX0 + 16] = _cols(g["norm_mix"][0])
    vec[:, V_FFN0:V_FFN0 + 16] = _cols(g["norm_ffn"][0])
    vec[:, V_PLE0:V_PLE0 + 16] = _cols(g["norm_ple"][0])
    vec[:, V_KV:V_KV + 16] = _cols(g["kv_norm"])
    vec[:, V_MIX1:V_MIX1 + 16] = _cols(g["norm_mix"][1])
    vec[:, V_FFN1:V_FFN1 + 16] = _cols(g["norm_ffn"][1])
    vec[:, V_PLE1:V_PLE1 + 16] = _cols(g["norm_ple"][1])
    for i in range(3):
        vec[:, V_CW + 16 * i:V_CW + 16 * (i + 1)] = _cols(g["conv_dw"][0, i])
    vec[:, V_KN] = g["k_norm"]
    vec[:, V_QN] = g["q_norm"][0]
    vec[:, V_BF:V_BF + 16] = g["b_f"][None, :]
    sh["vecs"] = vec
    return sh, g


def core_inputs(sh, g, c):
    b, hf = c // 2, c % 2
    x, p = g["x"], g["p"]
    xt = np.zeros((128, KC, 2 * TN), np.float32)
    p0 = np.zeros((128, 2, 2 * TN), np.float32)
    own = slice(hf * TN, (hf + 1) * TN)
    xt[:, :, TN:] = _fm(x[b, own])
    p0[:, :, TN:] = _fm(p[0, b, own])
    if hf == 1:
        xt[:, :, :TN] = _fm(x[b, :TN])
        p0[:, :, :TN] = _fm(p[0, b, :TN])
    m = dict(sh)
    vec = sh["vecs"].copy()
    vec[:, V_VIS] = 0.0 if hf == 1 else -30000.0
    m["vecs"] = vec
    m["xT"] = xt
    m["pT0"] = p0
    m["pT1"] = _fm(p[1, b, own])
    return m


def kernel(**inputs):
    sh, g = prep_shared(inputs)
    nc = build_nc()
    in_maps = [core_inputs(sh, g, c) for c in range(8)]
    res = run_bass_kernel_spmd(nc, in_maps, core_ids=list(range(8)))
    outp = np.zeros((4, 2048, D), np.float32)
    for c in range(8):
        o = res.results[c]["out"]
        outp[c // 2, (c % 2) * TN:(c % 2 + 1) * TN, :] = o.transpose(2, 1, 0).reshape(TN, D)
    return outp
```

```python
import numpy as np
from contextlib import ExitStack
import concourse.bass as bass
import concourse.mybir as mybir
from concourse.bass_utils import run_bass_kernel_spmd

F32 = mybir.dt.float32
BF16 = mybir.dt.bfloat16
ALU = mybir.AluOpType
AF = mybir.ActivationFunctionType
AX = mybir.AxisListType

D = 2048
KC = 16
TN = 1024
NH = 16
DFF = 5632
FQ = 11
NE = 8
DFE = 7168
EQ = 14
NS = 5
NTMP = 5
EPS = 1e-6
SCALE = 128 ** -0.5
NVEC = 179
V_MIX0, V_FFN0, V_PLE0, V_KV, V_MIX1, V_FFN1, V_PLE1, V_CW, V_KN, V_QN, V_VIS, V_BF = 0, 16, 32, 48, 64, 80, 96, 112, 160, 161, 162, 163


class Buf:
    __slots__ = ("name", "w", "rs")

    def __init__(self, name):
        self.name = name
        self.w = None
        self.rs = []


class Trk:
    def __init__(self, nc, stack):
        self.nc = nc
        self.stack = stack
        self.engs = {"pe": nc.tensor, "act": nc.scalar, "dve": nc.vector, "pool": nc.gpsimd, "sp": nc.sync}
        self.sem = {}
        self.cnt = {}
        self.owner = {}
        self.seen = {k: {} for k in self.engs}
        self.nsem = 0
        for k in self.engs:
            self._new_sem(k)
        self.dsem = {}

    def _new_sem(self, k):
        s = self.stack.enter_context(self.nc.semaphore(f"s_{k}_{self.nsem}"))
        self.nsem += 1
        self.sem[k] = s
        self.cnt[k] = 0
        self.owner[id(s)] = k

    def roll(self):
        for k in ("pe", "act", "dve"):
            self._new_sem(k)

    def _waits(self, e, reads, writes):
        need = {}

        def add(st):
            if st is None:
                return
            s, v = st
            if e == "pe" and self.owner.get(id(s)) == "pe":
                return
            key = id(s)
            if key not in need or need[key][1] < v:
                need[key] = (s, v)
        for b in reads:
            add(b.w)
        for b in writes:
            add(b.w)
            for r in b.rs:
                add(r)
        eng = self.engs[e]
        seen = self.seen[e]
        for key, (s, v) in need.items():
            if seen.get(key, 0) < v:
                eng.wait_ge(s, v)
                seen[key] = v

    @staticmethod
    def _stamp(st, reads, writes):
        for b in reads:
            b.rs.append(st)
            if len(b.rs) > 64:
                last = {}
                for s, v in b.rs:
                    if id(s) not in last or last[id(s)][1] < v:
                        last[id(s)] = (s, v)
                b.rs = list(last.values())
        for b in writes:
            b.w = st
            b.rs = []

    def op(self, e, fn, reads=(), writes=()):
        self._waits(e, reads, writes)
        ins = fn(self.engs[e])
        self.cnt[e] += 1
        ins.then_inc(self.sem[e], 1)
        self._stamp((self.sem[e], self.cnt[e]), reads, writes)

    def dma(self, e, key, out, in_, reads=(), writes=()):
        if key not in self.dsem:
            s = self.stack.enter_context(self.nc.semaphore(f"d_{key}"))
            self.dsem[key] = [s, 0]
        s, c = self.dsem[key]
        eng = self.engs[e]
        seen = self.seen[e]
        if c > 0 and seen.get(id(s), 0) < c:
            eng.wait_ge(s, c)
            seen[id(s)] = c
        self._waits(e, reads, writes)
        eng.dma_start(out=out, in_=in_).then_inc(s, 16)
        self.dsem[key][1] = c + 16
        self._stamp((s, c + 16), reads, writes)

    def finish(self, e, bufs):
        self._waits(e, bufs, ())


def build_nc(n_experts=NE, dbg=False):
    nc = bass.Bass("TRN2", target_bir_lowering=False)

    def din(name, shape, dt=F32):
        return nc.dram_tensor(name, list(shape), dt, kind="ExternalInput").ap()

    xT = din("xT", [128, KC, 2 * TN])
    pT0 = din("pT0", [128, 2, 2 * TN])
    pT1 = din("pT1", [128, 2, TN])
    vecs = din("vecs", [128, NVEC])
    consts = din("consts", [128, 3, 128])
    wr = din("wr", [128, KC, NE])
    w_cin = din("w_cin", [48, 128, KC, 128])
    w_cout = din("w_cout", [16, 128, KC, 128])
    w_gu = din("w_gu", [88, 128, KC, 128])
    w_dn = din("w_dn", [4, 16, 128, FQ, 128])
    w_pg = din("w_pg", [2, 16, 128, KC, 128])
    w_pu = din("w_pu", [2, 16, 128, 2, 128])
    w_k = din("w_k", [16, 128, KC, 128])
    w_v = din("w_v", [16, 128, KC, 128])
    w_f = din("w_f", [128, KC, 16])
    w_q = din("w_q", [16, 128, KC, 128])
    w_o = din("w_o", [16, 128, KC, 128])
    if n_experts > 0:
        w_mgu = din("w_mgu", [NE, 112, 128, KC, 128])
        w_mdn = din("w_mdn", [NE, 4, 16, 128, EQ, 128])
    out = nc.dram_tensor("out", [128, KC, TN], F32, kind="ExternalOutput").ap()
    ktd = nc.dram_tensor("ktd", [NH, 128, 2 * TN], BF16, kind="Internal").ap()
    vd = nc.dram_tensor("vd", [NH, 128, 16, 128], BF16, kind="Internal").ap()
    if dbg:
        dbg_o = nc.dram_tensor("dbg", [4, 128, KC, TN], F32, kind="ExternalOutput").ap()

    with ExitStack() as st:
        def sb(name, shape, dt):
            return st.enter_context(nc.sbuf_tensor(name, list(shape), dt))

        H = sb("H", [128, KC, TN], F32)
        XN = sb("XN", [128, KC, TN], BF16)
        BIG = sb("BIG", [128, KC, TN], BF16)
        WS = sb("WS", [128, NS, KC * 128], BF16)
        SCR = sb("SCR", [128, 12288], BF16)
        TMP = sb("TMP", [128, NTMP, TN + 2], F32)
        VEC = sb("VEC", [128, NVEC], F32)
        WR = sb("WR", [128, KC, NE], F32)
        CON = sb("CON", [128, 3, 128], F32)
        CMD = sb("CMD", [128, 128], F32)
        CMH = sb("CMH", [128, 128], F32)
        ONESF = sb("ONESF", [128, 128], F32)
        ONESB = sb("ONESB", [128, 128], BF16)
        CMDB = sb("CMDB", [128, 128], BF16)
        TRIB = sb("TRIB", [128, 128], BF16)
        EPSC = sb("EPSC", [128, 1], F32)
        HALO = sb("HALO", [128, KC, 2], F32)
        LS = sb("LS", [128, 16, 16], F32)
        CC = sb("CC", [128, 16, 16], F32)
        BIASQ = sb("BIASQ", [128, 2, 16, 16], F32)
        FL = sb("FL", [128, 8, 16], F32)
        LG = sb("LG", [128, 8, 8], F32)
        LG2 = sb("LG2", [128, 8, 8], F32)
        EQ1 = sb("EQ1", [128, 8, 8], F32)
        EQ2 = sb("EQ2", [128, 8, 8], F32)
        GT = sb("GT", [128, 8, 8], F32)
        MM = sb("MM", [128, 8, 4], F32)
        DG = sb("DG", [128, 2, 128], F32)
        PS = st.enter_context(nc.psum_tensor("PS", [128, 4096], F32))

        IDENT, TRI, SEL = CON[:, 0, :], CON[:, 1, :], CON[:, 2, :]
        tk = Trk(nc, st)

        bH = [Buf(f"H{j}") for j in range(KC)]
        bXN = [Buf(f"XN{j}") for j in range(KC)]
        bBIG = [Buf(f"BIG{j}") for j in range(KC)]
        bWS = [Buf(f"WS{s}") for s in range(NS)]
        bT = [Buf(f"T{i}") for i in range(NTMP)]
        bP = [Buf(f"P{i}") for i in range(8)]
        bVEC, bWR, bCON, bCST, bHALO, bLS, bCC, bBQ, bFL = (Buf(n) for n in ("VEC", "WR", "CON", "CST", "HALO", "LS", "CC", "BQ", "FL"))
        bSCR = [Buf(f"SCR{i}") for i in range(12)]
        bKD, bVD, bOUT, bDBG, bRT, bDG = Buf("KD"), Buf("VD"), Buf("OUT"), Buf("DBG"), Buf("RT"), [Buf("DG0"), Buf("DG1")]

        st_ = {"wi": 0, "pi": 0, "bi": 0, "ti": 0}

        def wload(src, kc, width=128):
            s = st_["wi"] % NS
            st_["wi"] += 1
            dst = WS[:, s, 0:kc * width].rearrange("p (k m) -> p k m", k=kc)
            tk.dma("pool", f"ws{s}", dst, src, writes=[bWS[s]])
            return s

        def wsv(s, k, width=128):
            return WS[:, s, k * width:(k + 1) * width]

        def pair(n=3):
            p = st_["pi"] % n
            st_["pi"] += 1
            return p

        def bank(n=6):
            b = st_["bi"] % n
            st_["bi"] += 1
            return b

        def tmp():
            t = st_["ti"] % NTMP
            st_["ti"] += 1
            return t

        def PP(p):
            return PS[:, p * 1024:(p + 1) * 1024]

        def PB(b, lo=0, hi=512):
            return PS[:, b * 512 + lo:b * 512 + hi]

        def bPP(p):
            return [bP[2 * p], bP[2 * p + 1]]

        def T(t, lo=0, hi=TN):
            return TMP[:, t, lo:hi]

        def vcol(c):
            return VEC[:, c:c + 1]

        def fm_group(p, s, kc, rhs, reads):
            def f(e):
                ins = None
                for half in range(2):
                    for k in range(kc):
                        ins = e.matmul(PB(2 * p + half), wsv(s, k), rhs(k, half), start=(k == 0), stop=(k == kc - 1))
                return ins
            tk.op("pe", f, reads=[bWS[s]] + list(reads), writes=bPP(p))

        def xn_rhs(k, half):
            return XN[:, k, half * 512:(half + 1) * 512]

        def big_rhs(k, half):
            return BIG[:, k, half * 512:(half + 1) * 512]

        def bcast_mean(p, src_t, cm):
            def f(e):
                ins = None
                for half in range(2):
                    ins = e.matmul(PB(2 * p + half), cm[:], T(src_t, half * 512, (half + 1) * 512), start=True, stop=True)
                return ins
            tk.op("pe", f, reads=[bT[src_t], bCST], writes=bPP(p))

        def rstd_from(p):
            r = tmp()
            tk.op("act", lambda e: e.activation(out=T(r), in_=PP(p), func=AF.Sqrt, bias=EPSC[:, 0:1], scale=1.0),
                  reads=bPP(p) + [bCST], writes=[bT[r]])
            tk.op("dve", lambda e: e.reciprocal(out=T(r), in_=T(r)), reads=[bT[r]], writes=[bT[r]])
            return r

        def rmsnorm(gc, router=False, sq_big=True):
            p = pair(2) if sq_big else pair()
            for j in range(KC):
                if sq_big:
                    c = 14 + (j % 2)
                    tk.op("act", lambda e: e.activation(out=BIG[:, c, :], in_=H[:, j, :], func=AF.Square), reads=[bH[j]], writes=[bBIG[c]])

                    def f(e):
                        ins = None
                        for half in range(2):
                            ins = e.matmul(PB(2 * p + half), CMDB[:], BIG[:, c, half * 512:(half + 1) * 512], start=(j == 0), stop=(j == KC - 1))
                        return ins
                    tk.op("pe", f, reads=[bBIG[c], bCST], writes=bPP(p))
                    continue
                t = tmp()
                tk.op("act", lambda e: e.activation(out=T(t), in_=H[:, j, :], func=AF.Square), reads=[bH[j]], writes=[bT[t]])

                def f(e):
                    ins = None
                    for half in range(2):
                        ins = e.matmul(PB(2 * p + half), CMD[:], T(t, half * 512, (half + 1) * 512), start=(j == 0), stop=(j == KC - 1))
                    return ins
                tk.op("pe", f, reads=[bT[t], bCST], writes=bPP(p))
            r = rstd_from(p)
            if not router:
                for j in range(KC):
                    tk.op("dve", lambda e: e.scalar_tensor_tensor(out=XN[:, j, :], in0=H[:, j, :], scalar=vcol(gc + j), in1=T(r),
                                                                   op0=ALU.mult, op1=ALU.mult),
                          reads=[bH[j], bVEC, bT[r]], writes=[bXN[j]])
                return None
            lb = 7
            for j in range(KC):
                t = tmp()
                if t == r:
                    t = tmp()
                tk.op("dve", lambda e: e.scalar_tensor_tensor(out=T(t), in0=H[:, j, :], scalar=vcol(gc + j), in1=T(r),
                                                               op0=ALU.mult, op1=ALU.mult),
                      reads=[bH[j], bVEC, bT[r]], writes=[bT[t]])
                tk.op("act", lambda e: e.activation(out=XN[:, j, :], in_=T(t), func=AF.Copy), reads=[bT[t]], writes=[bXN[j]])

                def f(e):
                    ins = None
                    for tb in range(8):
                        ins = e.matmul(PB(lb, tb * 8, tb * 8 + 8), T(t, tb * 128, (tb + 1) * 128), WR[:, j, :],
                                       start=(j == 0 and tb == 0), stop=(j == KC - 1), skip_group_check=True)
                    return ins
                tk.op("pe", f, reads=[bT[t], bWR], writes=[bP[lb]])
            return lb

        def add_into_H(j, p):
            tk.op("dve", lambda e: e.tensor_tensor(out=H[:, j, :], in0=PP(p), in1=H[:, j, :], op=ALU.add), reads=bPP(p) + [bH[j]], writes=[bH[j]])

        def barrier():
            stamps = [(tk.sem[k], tk.cnt[k]) for k in ("pe", "act", "dve") if tk.cnt[k] > 0]
            stamps += [(s, c) for (s, c) in tk.dsem.values() if c > 0]
            fake = Buf("bar")
            for e in ("pe", "act", "dve", "sp", "pool"):
                for stp in stamps:
                    fake.w = stp
                    tk._waits(e, [fake], ())

        tk.dma("sp", "vec", VEC[:], vecs, writes=[bVEC])
        tk.dma("sp", "con", CON[:], consts, writes=[bCON])
        tk.dma("sp", "wr", WR[:], wr, writes=[bWR])
        tk.op("dve", lambda e: e.memset(CMD[:], 1.0 / D), writes=[bCST])
        tk.op("dve", lambda e: e.memset(CMH[:], 1.0 / 128), writes=[bCST])
        tk.op("dve", lambda e: e.memset(ONESF[:], 1.0), writes=[bCST])
        tk.op("dve", lambda e: e.memset(ONESB[:], 1.0), writes=[bCST])
        tk.op("dve", lambda e: e.memset(CMDB[:], 1.0 / D), writes=[bCST])
        tk.op("dve", lambda e: e.memset(EPSC[:], EPS), writes=[bCST])
        tk.op("dve", lambda e: e.memset(HALO[:], 0.0), writes=[bHALO])
        tk.op("dve", lambda e: e.tensor_copy(out=TRIB[:], in_=TRI), reads=[bCON], writes=[bCST])

        PT = SCR[:, 0:2048].rearrange("p (a b) -> p a b", a=2)

        def ple(layer, gc):
            rmsnorm(gc)
            for j in range(KC):
                sg = wload(w_pg[layer, j], KC)
                su = wload(w_pu[layer, j], 2)
                pa, pb = pair(), pair()
                fm_group(pa, sg, KC, xn_rhs, bXN)
                fm_group(pb, su, 2, lambda k, half: PT[:, k, half * 512:(half + 1) * 512], [bSCR[0], bSCR[1]])
                a, b = tmp(), tmp()
                tk.op("act", lambda e: e.activation(out=T(a), in_=PP(pa), func=AF.Sigmoid), reads=bPP(pa), writes=[bT[a]])
                tk.op("dve", lambda e: e.tensor_tensor(out=T(b), in0=T(a), in1=PP(pb), op=ALU.mult), reads=[bT[a]] + bPP(pb), writes=[bT[b]])
                tk.op("dve", lambda e: e.tensor_tensor(out=H[:, j, :], in0=T(b), in1=H[:, j, :], op=ALU.add), reads=[bT[b], bH[j]], writes=[bH[j]])

        def layer0_pass(ps_):
            c0 = ps_ * TN
            for j in range(KC):
                tk.dma("sp", f"h{j % 4}", H[:, j, :], xT[:, j, c0:c0 + TN], writes=[bH[j]])
            tk.dma("pool", "pt", PT, pT0[:, :, c0:c0 + TN], writes=[bSCR[0], bSCR[1]])
            rmsnorm(V_MIX0)
            for j in range(KC):
                sC = wload(w_cin[16 + j], KC)
                sX = wload(w_cin[32 + j], KC)
                sB = wload(w_cin[j], KC)
                pC, pX, pBg = pair(), pair(), pair()
                fm_group(pC, sC, KC, xn_rhs, bXN)
                fm_group(pX, sX, KC, xn_rhs, bXN)
                fm_group(pBg, sB, KC, xn_rhs, bXN)
                a, u, acc = tmp(), tmp(), tmp()
                tk.op("act", lambda e: e.activation(out=T(a), in_=PP(pC), func=AF.Copy), reads=bPP(pC), writes=[bT[a]])
                tk.op("dve", lambda e: e.tensor_copy(out=TMP[:, u, 0:2], in_=HALO[:, j, :]), reads=[bHALO], writes=[bT[u]])
                tk.op("dve", lambda e: e.tensor_tensor(out=TMP[:, u, 2:TN + 2], in0=T(a), in1=PP(pX), op=ALU.mult),
                      reads=[bT[a]] + bPP(pX), writes=[bT[u]])
                tk.op("dve", lambda e: e.tensor_copy(out=HALO[:, j, :], in_=TMP[:, u, TN:TN + 2]), reads=[bT[u]], writes=[bHALO])
                tk.op("dve", lambda e: e.tensor_scalar(out=T(acc), in0=TMP[:, u, 0:TN], scalar1=vcol(V_CW + j), scalar2=None, op0=ALU.mult),
                      reads=[bT[u], bVEC], writes=[bT[acc]])
                for i in (1, 2):
                    tk.op("dve", lambda e: e.scalar_tensor_tensor(out=T(acc), in0=TMP[:, u, i:TN + i], scalar=vcol(V_CW + 16 * i + j), in1=T(acc),
                                                                   op0=ALU.mult, op1=ALU.add),
                          reads=[bT[u], bVEC, bT[acc]], writes=[bT[acc]])
                tk.op("dve", lambda e: e.tensor_tensor(out=BIG[:, j, :], in0=T(acc), in1=PP(pBg), op=ALU.mult),
                      reads=[bT[acc]] + bPP(pBg), writes=[bBIG[j]])
            for j in range(KC):
                s = wload(w_cout[j], KC)
                p = pair()
                fm_group(p, s, KC, big_rhs, bBIG)
                add_into_H(j, p)
            rmsnorm(V_FFN0)
            for q in range(4):
                for fi in range(FQ):
                    f = q * FQ + fi
                    sg = wload(w_gu[f], KC)
                    su = wload(w_gu[44 + f], KC)
                    pg, pu = pair(), pair()
                    fm_group(pg, sg, KC, xn_rhs, bXN)
                    fm_group(pu, su, KC, xn_rhs, bXN)
                    a = tmp()
                    tk.op("act", lambda e: e.activation(out=T(a), in_=PP(pg), func=AF.Silu), reads=bPP(pg), writes=[bT[a]])
                    tk.op("dve", lambda e: e.tensor_tensor(out=BIG[:, fi, :], in0=T(a), in1=PP(pu), op=ALU.mult),
                          reads=[bT[a]] + bPP(pu), writes=[bBIG[fi]])
                for j in range(KC):
                    s = wload(w_dn[q, j], FQ)
                    p = pair()
                    fm_group(p, s, FQ, big_rhs, bBIG[:FQ])
                    add_into_H(j, p)
            ple(0, V_PLE0)
            if dbg:
                tk.dma("sp", "dbg", dbg_o[ps_], H[:], reads=bH, writes=[bDBG])
            rmsnorm(V_KV)
            for hd in range(NH):
                s = wload(w_k[hd], KC)
                p = pair()
                fm_group(p, s, KC, xn_rhs, bXN)
                a = tmp()
                tk.op("act", lambda e: e.activation(out=T(a), in_=PP(p), func=AF.Square), reads=bPP(p), writes=[bT[a]])
                p2 = pair()
                bcast_mean(p2, a, CMH)
                r = rstd_from(p2)
                tk.op("dve", lambda e: e.scalar_tensor_tensor(out=BIG[:, hd, :], in0=PP(p), scalar=vcol(V_KN), in1=T(r), op0=ALU.mult, op1=ALU.mult),
                      reads=bPP(p) + [bVEC, bT[r]], writes=[bBIG[hd]])
                tk.dma("sp", f"ko{hd % 4}", ktd[hd, :, c0:c0 + TN], BIG[:, hd, :], reads=[bBIG[hd]], writes=[bKD])
            for j in range(NH):
                s = wload(w_v[j], KC)
                p = pair()

                def f(e):
                    ins = None
                    for tb in range(8):
                        for k in range(KC):
                            ins = e.matmul(PS[:, p * 1024 + tb * 128:p * 1024 + (tb + 1) * 128], XN[:, k, tb * 128:(tb + 1) * 128], wsv(s, k),
                                           start=(k == 0), stop=(k == KC - 1))
                    return ins
                tk.op("pe", f, reads=[bWS[s]] + bXN, writes=bPP(p))
                tk.op("act", lambda e: e.activation(out=BIG[:, j, :], in_=PP(p), func=AF.Copy), reads=bPP(p), writes=[bBIG[j]])
                tk.dma("sp", f"vo{j % 4}", vd[j, :, ps_ * 8:(ps_ + 1) * 8, :], BIG[:, j, :].rearrange("p (a b) -> p a b", a=8),
                       reads=[bBIG[j]], writes=[bVD])
            s = wload(w_f, KC, width=16)
            fb = 6

            def f(e):
                ins = None
                for tb in range(8):
                    for k in range(KC):
                        ins = e.matmul(PB(fb, tb * 16, tb * 16 + 16), XN[:, k, tb * 128:(tb + 1) * 128], wsv(s, k, 16), start=(k == 0), stop=(k == KC - 1))
                return ins
            tk.op("pe", f, reads=[bWS[s]] + bXN, writes=[bP[fb]])
            for tb in range(8):
                tk.op("dve", lambda e: e.tensor_tensor(out=FL[:, tb, :], in0=PB(fb, tb * 16, tb * 16 + 16), in1=VEC[:, V_BF:V_BF + 16], op=ALU.add),
                      reads=[bP[fb], bVEC], writes=[bFL])
            tk.op("act", lambda e: e.activation(out=FL[:], in_=FL[:], func=AF.Sigmoid), reads=[bFL], writes=[bFL])
            tk.op("act", lambda e: e.activation(out=LS[:, ps_ * 8:(ps_ + 1) * 8, :], in_=FL[:], func=AF.Ln), reads=[bFL], writes=[bLS])

        layer0_pass(0)
        tk.roll()
        layer0_pass(1)
        tk.roll()

        cb = 6
        for b in range(16):
            def f(e):
                ins = None
                for b2 in range(b + 1):
                    ins = e.matmul(PB(cb, 0, 16), (ONESF[:] if b2 < b else TRI), LS[:, b2, :], start=(b2 == 0), stop=(b2 == b))
                return ins
            tk.op("pe", f, reads=[bLS, bCST, bCON], writes=[bP[cb]])
            tk.op("act", lambda e: e.activation(out=CC[:, b, :], in_=PB(cb, 0, 16), func=AF.Copy), reads=[bP[cb]], writes=[bCC])
        for qt in range(2):
            lastb = 8 + 4 * qt + 3
            tk.op("pe", lambda e: e.matmul(PB(cb, 0, 16), SEL, CC[:, lastb, :], start=True, stop=True), reads=[bCC, bCON], writes=[bP[cb]])
            for kb in range(16):
                tk.op("dve", lambda e: e.tensor_tensor(out=BIASQ[:, qt, kb, :], in0=PB(cb, 0, 16), in1=CC[:, kb, :], op=ALU.subtract),
                      reads=[bP[cb], bCC], writes=[bBQ])
            tk.op("dve", lambda e: e.tensor_scalar(out=BIASQ[:, qt, 0:8, :], in0=BIASQ[:, qt, 0:8, :], scalar1=vcol(V_VIS), scalar2=None, op0=ALU.add),
                  reads=[bBQ, bVEC], writes=[bBQ])
        rmsnorm(V_MIX1)
        barrier()

        def KTH(i):
            return SCR[:, i * 2048:(i + 1) * 2048]

        def VH(i):
            return SCR[:, 4096 + i * 2048:4096 + (i + 1) * 2048]

        def QTH(i):
            return SCR[:, 8192 + i * 1024:8192 + (i + 1) * 1024]

        def PTL(i):
            return SCR[:, 10240 + i * 512:10240 + (i + 1) * 512]

        bKTH = [[bSCR[0], bSCR[1]], [bSCR[2], bSCR[3]]]
        bVH = [[bSCR[4], bSCR[5]], [bSCR[6], bSCR[7]]]
        bQTH = [[bSCR[8]], [bSCR[9]]]
        bPTL = [Buf(f"PT{i}") for i in range(4)]
        def prologue(h):
            i2 = h % 2
            s = wload(w_q[h], KC)
            p = pair(2)
            fm_group(p, s, KC, xn_rhs, bXN)
            a = tmp()
            tk.op("act", lambda e: e.activation(out=T(a), in_=PP(p), func=AF.Square), reads=bPP(p), writes=[bT[a]])
            p2 = pair(2)
            bcast_mean(p2, a, CMH)
            r = rstd_from(p2)
            tk.op("dve", lambda e: e.scalar_tensor_tensor(out=QTH(i2), in0=PP(p), scalar=vcol(V_QN), in1=T(r), op0=ALU.mult, op1=ALU.mult),
                  reads=bPP(p) + [bVEC, bT[r]], writes=bQTH[i2])
            tk.dma("sp", f"kl{i2}", KTH(i2), ktd[h], reads=[bKD], writes=bKTH[i2])
            tk.dma("sp", f"vl{i2}", VH(i2).rearrange("p (a b) -> p a b", a=16), vd[h], reads=[bVD], writes=bVH[i2])

        tiles = [(h, qt, kb) for h in range(NH) for qt in range(2) for kb in range(8 + 4 * qt + 4)]
        tinfo = {}
        LOOK = 2

        def geom(qt, kb):
            jd = kb - (8 + 4 * qt)
            return jd, (128 * jd if jd >= 0 else 0)

        def emit_qk(i):
            h, qt, kb = tiles[i]
            i2 = h % 2
            if qt == 1 and kb == 0 and h + 1 < NH:
                prologue(h + 1)
            jd, lo = geom(qt, kb)
            sbk = bank(4)
            pt = i % 4
            tinfo[i] = pt
            tk.op("pe", lambda e: e.matmul(PB(sbk, lo, 512), KTH(i2)[:, kb * 128:(kb + 1) * 128], QTH(i2)[:, qt * 512 + lo:(qt + 1) * 512],
                                           start=True, stop=True),
                  reads=bKTH[i2] + bQTH[i2], writes=[bP[sbk]])
            tk.op("act", lambda e: e.activation(out=PTL(pt)[:, lo:512], in_=PB(sbk, lo, 512), func=AF.Exp,
                                                bias=BIASQ[:, qt, kb, h:h + 1], scale=SCALE),
                  reads=[bP[sbk], bBQ], writes=[bPTL[pt]])
            if jd >= 0:
                tk.op("dve", lambda e: e.tensor_tensor(out=PTL(pt)[:, lo:lo + 128], in0=PTL(pt)[:, lo:lo + 128], in1=TRIB[:], op=ALU.mult),
                      reads=[bPTL[pt], bCST], writes=[bPTL[pt]])

        def emit_pv(i):
            h, qt, kb = tiles[i]
            i2 = h % 2
            nkb = 8 + 4 * qt + 4
            jd, lo = geom(qt, kb)
            pt = tinfo.pop(i)
            g = (2 * h + qt) % 2
            OB, LB = 4 + 2 * g, 5 + 2 * g

            def f(e):
                e.matmul(PB(OB, lo, 512), VH(i2)[:, kb * 128:(kb + 1) * 128], PTL(pt)[:, lo:512], start=(kb == 0), stop=(kb == nkb - 1))
                return e.matmul(PB(LB, lo, 512), ONESB[:], PTL(pt)[:, lo:512], start=(kb == 0), stop=(kb == nkb - 1))
            tk.op("pe", f, reads=bVH[i2] + [bPTL[pt], bCST], writes=[bP[OB], bP[LB]])
            if kb == nkb - 1:
                r = tmp()
                tk.op("dve", lambda e: e.reciprocal(out=T(r, 0, 512), in_=PB(LB)), reads=[bP[LB]], writes=[bT[r]])
                tk.op("dve", lambda e: e.tensor_tensor(out=BIG[:, h, qt * 512:(qt + 1) * 512], in0=PB(OB), in1=T(r, 0, 512), op=ALU.mult),
                      reads=[bP[OB], bT[r]], writes=[bBIG[h]])

        prologue(0)
        for i in range(len(tiles) + LOOK):
            if i < len(tiles):
                emit_qk(i)
            if i >= LOOK:
                emit_pv(i - LOOK)
        for j in range(KC):
            s = wload(w_o[j], KC)
            p = pair()
            fm_group(p, s, KC, big_rhs, bBIG)
            add_into_H(j, p)
        if dbg:
            tk.dma("sp", "dbg", dbg_o[2], H[:], reads=bH, writes=[bDBG])
        tk.roll()
        barrier()

        lb = rmsnorm(V_FFN1, router=True)
        tk.op("act", lambda e: e.activation(out=LG[:], in_=PB(lb, 0, 64).rearrange("p (a b) -> p a b", a=8), func=AF.Copy), reads=[bP[lb]], writes=[bRT])
        for tb in range(8):
            def rt(fn):
                tk.op("dve", fn, reads=[bRT], writes=[bRT])
            rt(lambda e: e.tensor_reduce(out=MM[:, tb, 0:1], in_=LG[:, tb, :], axis=AX.X, op=ALU.max))
            rt(lambda e: e.tensor_scalar(out=EQ1[:, tb, :], in0=LG[:, tb, :], scalar1=MM[:, tb, 0:1], scalar2=None, op0=ALU.is_equal))
            rt(lambda e: e.scalar_tensor_tensor(out=LG2[:, tb, :], in0=EQ1[:, tb, :], scalar=-1e30, in1=LG[:, tb, :], op0=ALU.mult, op1=ALU.add))
            rt(lambda e: e.tensor_reduce(out=MM[:, tb, 1:2], in_=LG2[:, tb, :], axis=AX.X, op=ALU.max))
            rt(lambda e: e.tensor_scalar(out=EQ2[:, tb, :], in0=LG2[:, tb, :], scalar1=MM[:, tb, 1:2], scalar2=None, op0=ALU.is_equal))
            rt(lambda e: e.tensor_tensor(out=MM[:, tb, 2:3], in0=MM[:, tb, 1:2], in1=MM[:, tb, 0:1], op=ALU.subtract))
        tk.op("act", lambda e: e.activation(out=MM[:, :, 2:3], in_=MM[:, :, 2:3], func=AF.Sigmoid), reads=[bRT], writes=[bRT])
        tk.op("dve", lambda e: e.tensor_scalar(out=MM[:, :, 3:4], in0=MM[:, :, 2:3], scalar1=-1.0, scalar2=1.0, op0=ALU.mult, op1=ALU.add),
              reads=[bRT], writes=[bRT])
        for tb in range(8):
            tk.op("dve", lambda e: e.tensor_scalar(out=GT[:, tb, :], in0=EQ1[:, tb, :], scalar1=MM[:, tb, 3:4], scalar2=None, op0=ALU.mult),
                  reads=[bRT], writes=[bRT])
            tk.op("dve", lambda e: e.scalar_tensor_tensor(out=GT[:, tb, :], in0=EQ2[:, tb, :], scalar=MM[:, tb, 2:3], in1=GT[:, tb, :],
                                                           op0=ALU.mult, op1=ALU.add),
                  reads=[bRT], writes=[bRT])

        def GATE(e_):
            return SCR[:, e_ * 1024:(e_ + 1) * 1024]

        di = 0
        for e_ in range(n_experts):
            p = pair()
            for tb in range(8):
                d_ = di % 2
                di += 1
                tk.op("dve", lambda e: e.tensor_scalar(out=DG[:, d_, :], in0=IDENT, scalar1=GT[:, tb, e_:e_ + 1], scalar2=None, op0=ALU.mult),
                      reads=[bRT, bCON], writes=[bDG[d_]])
                tk.op("pe", lambda e: e.matmul(PS[:, p * 1024 + tb * 128:p * 1024 + (tb + 1) * 128], ONESF[:], DG[:, d_, :], start=True, stop=True),
                      reads=[bDG[d_], bCST], writes=bPP(p))
            tk.op("act", lambda e: e.activation(out=GATE(e_), in_=PP(p), func=AF.Copy), reads=bPP(p), writes=[bSCR[e_]])
        for e_ in range(n_experts):
            for q in range(4):
                for fi in range(EQ):
                    f_ = q * EQ + fi
                    sg = wload(w_mgu[e_, f_], KC)
                    su = wload(w_mgu[e_, 56 + f_], KC)
                    pg, pu = pair(), pair()
                    fm_group(pg, sg, KC, xn_rhs, bXN)
                    fm_group(pu, su, KC, xn_rhs, bXN)
                    a, b = tmp(), tmp()
                    tk.op("act", lambda e: e.activation(out=T(a), in_=PP(pg), func=AF.Silu), reads=bPP(pg), writes=[bT[a]])
                    tk.op("dve", lambda e: e.tensor_tensor(out=T(b), in0=T(a), in1=PP(pu), op=ALU.mult), reads=[bT[a]] + bPP(pu), writes=[bT[b]])
                    tk.op("dve", lambda e: e.tensor_tensor(out=BIG[:, fi, :], in0=T(b), in1=GATE(e_), op=ALU.mult),
                          reads=[bT[b], bSCR[e_]], writes=[bBIG[fi]])
                for j in range(KC):
                    s = wload(w_mdn[e_, q, j], EQ)
                    p = pair()
                    fm_group(p, s, EQ, big_rhs, bBIG[:EQ])
                    add_into_H(j, p)
            tk.roll()
        if dbg:
            tk.dma("sp", "dbg", dbg_o[3], H[:], reads=bH, writes=[bDBG])
        barrier()

        tk.dma("pool", "pt", PT, pT1, writes=[bSCR[0], bSCR[1]])
        ple(1, V_PLE1)
        for j in range(KC):
            tk.dma("sp", f"h{j % 4}", out[:, j, :], H[:, j, :], reads=[bH[j]], writes=[bOUT])
        tk.finish("sp", [bOUT] + ([bDBG] if dbg else []))
    return nc


def _wtile(W):
    K, N = W.shape
    return np.ascontiguousarray(W.reshape(K // 128, 128, N // 128, 128).transpose(2, 1, 0, 3))


def _fm(a):
    t, f = a.shape
    return np.ascontiguousarray(a.T.reshape(f // 128, 128, t).transpose(1, 0, 2))


def _cols(v):
    return np.asarray(v, np.float32).reshape(-1, 128).T


def prep_shared(inp):
    f32 = np.float32
    g = {k: np.asarray(v, f32) for k, v in inp.items()}
    sh = {}
    sh["w_cin"] = _wtile(g["conv_in_w"][0])
    sh["w_cout"] = _wtile(g["conv_out_w"][0])
    sh["w_gu"] = _wtile(g["ffn_gu"][0])
    dn = g["ffn_down"][0]
    sh["w_dn"] = np.stack([_wtile(dn[q * FQ * 128:(q + 1) * FQ * 128]) for q in range(4)])
    sh["w_pg"] = np.stack([_wtile(g["ple_gate"][i]) for i in range(2)])
    sh["w_pu"] = np.stack([_wtile(g["ple_up"][i]) for i in range(2)])
    wkvf = g["w_kvf"]
    sh["w_k"] = _wtile(wkvf[:, :D])
    sh["w_v"] = _wtile(wkvf[:, D:2 * D])
    sh["w_f"] = np.ascontiguousarray(wkvf[:, 2 * D:].reshape(KC, 128, 16).transpose(1, 0, 2))
    sh["w_q"] = _wtile(g["attn_q_w"][0])
    sh["w_o"] = _wtile(g["attn_out_w"][0])
    sh["w_mgu"] = np.stack([_wtile(g["moe_gu"][0, e]) for e in range(NE)])
    md = g["moe_down"][0]
    sh["w_mdn"] = np.stack([np.stack([_wtile(md[e, q * EQ * 128:(q + 1) * EQ * 128]) for q in range(4)]) for e in range(NE)])
    sh["wr"] = np.ascontiguousarray(g["router_w"][0].reshape(KC, 128, NE).transpose(1, 0, 2))
    ident = np.eye(128, dtype=f32)
    tri = np.triu(np.ones((128, 128), f32))
    sel = np.zeros((128, 128), f32)
    sel[127, :] = 1.0
    sh["consts"] = np.ascontiguousarray(np.stack([ident, tri, sel], axis=1))
    vec = np.zeros((128, NVEC), f32)
    vec[:, V_MIX0:V_MIX0 + 16] = _cols(g["norm_mix"][0])
    vec[:, V_FFN0:V_FFN0 + 16] = _cols(g["norm_ffn"][0])
    vec[:, V_PLE0:V_PLE0 + 16] = _cols(g["norm_ple"][0])
    vec[:, V_KV:V_KV + 16] = _cols(g["kv_norm"])
    vec[:, V_MIX1:V_MIX1 + 16] = _cols(g["norm_mix"][1])
    vec[:, V_FFN1:V_FFN1 + 16] = _cols(g["norm_ffn"][1])
    vec[:, V_PLE1:V_PLE1 + 16] = _cols(g["norm_ple"][1])
    for i in range(3):
        vec[:, V_CW + 16 * i:V_CW + 16 * (i + 1)] = _cols(g["conv_dw"][0, i])
    vec[:, V_KN] = g["k_norm"]
    vec[:, V_QN] = g["q_norm"][0]
    vec[:, V_BF:V_BF + 16] = g["b_f"][None, :]
    sh["vecs"] = vec
    return sh, g


def core_inputs(sh, g, c):
    b, hf = c // 2, c % 2
    x, p = g["x"], g["p"]
    xt = np.zeros((128, KC, 2 * TN), np.float32)
    p0 = np.zeros((128, 2, 2 * TN), np.float32)
    own = slice(hf * TN, (hf + 1) * TN)
    xt[:, :, TN:] = _fm(x[b, own])
    p0[:, :, TN:] = _fm(p[0, b, own])
    if hf == 1:
        xt[:, :, :TN] = _fm(x[b, :TN])
        p0[:, :, :TN] = _fm(p[0, b, :TN])
    m = dict(sh)
    vec = sh["vecs"].copy()
    vec[:, V_VIS] = 0.0 if hf == 1 else -30000.0
    m["vecs"] = vec
    m["xT"] = xt
    m["pT0"] = p0
    m["pT1"] = _fm(p[1, b, own])
    return m


def kernel(**inputs):
    sh, g = prep_shared(inputs)
    nc = build_nc()
    in_maps = [core_inputs(sh, g, c) for c in range(8)]
    res = run_bass_kernel_spmd(nc, in_maps, core_ids=list(range(8)))
    outp = np.zeros((4, 2048, D), np.float32)
    for c in range(8):
        o = res.results[c]["out"]
        outp[c // 2, (c % 2) * TN:(c % 2 + 1) * TN, :] = o.transpose(2, 1, 0).reshape(TN, D)
    return outp
```
